# Optimizing a Trainium2 kernel written in Bass

```python
import jax, jax.numpy as jnp
from jax import lax
import numpy as np

D_MODEL = 2048
BATCH = 4
SEQ = 4096
DEPTH = 1

D_MIX = D_MODEL
D_LRU = D_MIX // 2
LRU_BLOCKS = 8
LRU_BLOCK = D_LRU // LRU_BLOCKS
CONV_WIDTH = 4
LRU_C = 8.0
D_HGRN = D_MIX - D_LRU
HGRN_EXPAND = 128
HGRN_HEADS = D_HGRN // HGRN_EXPAND
HGRN_DK = HGRN_EXPAND
HGRN_DV = D_HGRN // HGRN_HEADS
CHUNK = 64
IN_WIDTHS = (D_LRU, D_LRU, HGRN_HEADS * HGRN_DK, HGRN_HEADS * HGRN_DK, D_HGRN, D_HGRN)
IN_COLS = sum(IN_WIDTHS)
N_EXPERTS = 256
TOP_K = 8
N_GROUPS = 8
TOPK_GROUPS = 4
D_EXPERT = D_MODEL // 4
ROUTED_SCALE = 2.5
EXPERT_BLOCK = 128
RMS_EPS = 1e-6

kernel_name = "hybrid_rglru_hgrn2_moe_adaln"


def rms_norm(x, g):
    x32 = x.astype(jnp.float32)
    y = x32 * lax.rsqrt(jnp.mean(x32 * x32, axis=-1, keepdims=True) + RMS_EPS)
    return (y * g.astype(jnp.float32)).astype(x.dtype)


def rg_lru_group(u, z, conv_w, conv_b, wa, ba, wx, bx, lam):
    B, L, C = u.shape
    f32 = jnp.float32
    xc = lax.conv_general_dilated(u, conv_w[:, None, :], window_strides=(1,),
                                  padding=[(CONV_WIDTH - 1, 0)],
                                  dimension_numbers=('NWC', 'WIO', 'NWC'),
                                  feature_group_count=C) + conv_b
    xb = xc.reshape(B, L, LRU_BLOCKS, LRU_BLOCK)
    r = jax.nn.sigmoid((jnp.einsum('blhi,hij->blhj', xb, wa).reshape(B, L, C) + ba).astype(f32))
    i = jax.nn.sigmoid((jnp.einsum('blhi,hij->blhj', xb, wx).reshape(B, L, C) + bx).astype(f32))
    log_a = -LRU_C * r * jax.nn.softplus(-lam.astype(f32))
    a = jnp.exp(log_a)
    mult = jnp.sqrt(1.0 - jnp.exp(2.0 * log_a))
    mult = jnp.where((jnp.arange(L) == 0)[None, :, None], 1.0, mult)
    b = mult * i * xc.astype(f32)

    def combine(lhs, rhs):
        a1, b1 = lhs
        a2, b2 = rhs
        return a1 * a2, a2 * b1 + b2

    _, h = lax.associative_scan(combine, (a, b), axis=1)
    return (h * jax.nn.gelu(z.astype(f32))).astype(u.dtype)


def chunked_gated_recurrence(q, k, v, log_f):
    B, L, H, dk = q.shape
    dv = v.shape[-1]
    N = L // CHUNK

    def to_chunks(t):
        return t.reshape(B, N, CHUNK, H, t.shape[-1]).transpose(1, 0, 3, 2, 4)

    qc, kc, vc, gc = to_chunks(q), to_chunks(k), to_chunks(v), to_chunks(log_f)
    causal = jnp.tril(jnp.ones((CHUNK, CHUNK), bool))[:, :, None]

    def step(S, inp):
        q_, k_, v_, g_ = inp
        bcum = jnp.cumsum(g_, axis=2)
        o_inter = jnp.einsum('bhtk,bhkv->bhtv', q_ * jnp.exp(bcum), S)
        diff = bcum[:, :, :, None, :] - bcum[:, :, None, :, :]
        decay = jnp.exp(jnp.where(causal, diff, -jnp.inf))
        scores = jnp.sum(q_[:, :, :, None, :] * decay * k_[:, :, None, :, :], axis=-1)
        o_intra = jnp.einsum('bhts,bhsv->bhtv', scores, v_)
        b_last = bcum[:, :, -1:, :]
        S_new = jnp.exp(b_last[:, :, 0, :])[..., None] * S + \
            jnp.einsum('bhsk,bhsv->bhkv', k_ * jnp.exp(b_last - bcum), v_)
        return S_new, o_inter + o_intra

    S0 = jnp.zeros((B, H, dk, dv), q.dtype)
    _, o = lax.scan(step, S0, (qc, kc, vc, gc))
    return o.transpose(1, 0, 3, 2, 4).reshape(B, L, H, dv)


def hgrn2_group(q_in, f_in, v_in, g_in, lb, norm_g):
    B, L, _ = q_in.shape
    f32 = jnp.float32
    q = jax.nn.silu(q_in.astype(f32)).reshape(B, L, HGRN_HEADS, HGRN_DK)
    f = lb + (1.0 - lb) * jax.nn.sigmoid(f_in.astype(f32))
    log_f = jnp.log(f).reshape(B, L, HGRN_HEADS, HGRN_DK)
    k = (1.0 - f).reshape(B, L, HGRN_HEADS, HGRN_DK)
    v = v_in.astype(f32).reshape(B, L, HGRN_HEADS, HGRN_DV)
    o = chunked_gated_recurrence(q, k, v, log_f)
    o = o * lax.rsqrt(jnp.mean(o * o, axis=-1, keepdims=True) + RMS_EPS) * norm_g.astype(f32)
    o = o.reshape(B, L, D_HGRN) * jax.nn.silu(g_in.astype(f32))
    return o.astype(q_in.dtype)


def moe_ffn(h, w_router, router_bias, w_gate, w_up, w_down, ws_gate, ws_up, ws_down):
    B, L, D = h.shape
    T = B * L
    E, M, K = N_EXPERTS, EXPERT_BLOCK, TOP_K
    f32 = jnp.float32
    hf = h.reshape(T, D)
    scores = jax.nn.sigmoid(jnp.matmul(hf, w_router).astype(f32))
    sel = scores + router_bias.astype(f32)
    grp_score = jnp.sum(lax.top_k(sel.reshape(T, N_GROUPS, E // N_GROUPS), 2)[0], axis=-1)
    _, grp_idx = lax.top_k(grp_score, TOPK_GROUPS)
    grp_mask = jnp.any(grp_idx[:, :, None] == jnp.arange(N_GROUPS)[None, None, :], axis=1)
    sel = jnp.where(jnp.repeat(grp_mask, E // N_GROUPS, axis=1), sel, -jnp.inf)
    _, idx = lax.top_k(sel, K)
    w = jnp.take_along_axis(scores, idx, axis=1)
    w = w / jnp.sum(w, axis=-1, keepdims=True) * ROUTED_SCALE

    NK = T * K
    R = -(-(NK + E * (M - 1)) // M) * M
    NB = R // M
    flat_e = idx.reshape(-1)
    flat_tok = jnp.repeat(jnp.arange(T, dtype=jnp.int32), K)
    flat_w = w.reshape(-1)
    order = jnp.argsort(flat_e, stable=True)
    se = flat_e[order]
    counts = jnp.bincount(flat_e, length=E)
    starts = jnp.cumsum(counts) - counts
    pcounts = (counts + M - 1) // M * M
    pends = jnp.cumsum(pcounts)
    pstarts = pends - pcounts
    dest = pstarts[se] + jnp.arange(NK) - starts[se]
    row_tok = jnp.zeros((R,), jnp.int32).at[dest].set(flat_tok[order])
    row_w = jnp.zeros((R,), f32).at[dest].set(flat_w[order])
    block_e = jnp.minimum(jnp.searchsorted(pends, jnp.arange(NB) * M, side='right'), E - 1)

    def block_step(acc, blk):
        tok, wt, e = blk
        xb = hf[tok]
        a = jax.nn.silu(xb @ w_gate[e]) * (xb @ w_up[e])
        yb = (a @ w_down[e]).astype(f32) * wt[:, None]
        return acc.at[tok].add(yb), None

    routed, _ = lax.scan(block_step, jnp.zeros((T, D), f32),
                         (row_tok.reshape(NB, M), row_w.reshape(NB, M), block_e))
    shared = (jax.nn.silu(hf @ ws_gate) * (hf @ ws_up)) @ ws_down
    return (routed + shared.astype(f32)).astype(h.dtype).reshape(B, L, D)


def setup_inputs(seed: int = 0) -> dict:
    key = jax.random.key(seed)
    ks = jax.random.split(key, 26)
    f32 = jnp.float32
    D, E, F = D_MODEL, N_EXPERTS, D_EXPERT

    def nrm(k, shape, std):
        return jax.random.normal(k, shape, f32) * std

    u = jax.random.uniform(ks[12], (DEPTH, D_LRU), f32, 0.9, 0.999)
    a = u ** (1.0 / LRU_C)
    lru_lambda = jnp.log(a) - jnp.log1p(-a)
    return {
        "x": nrm(ks[0], (BATCH, SEQ, D), 1.0),
        "c": nrm(ks[1], (BATCH, D), 1.0),
        "w_ada": nrm(ks[2], (DEPTH, D, 6 * D), 0.5 * D ** -0.5),
        "b_ada": nrm(ks[3], (DEPTH, 6 * D), 0.02),
        "norm1_g": 1.0 + nrm(ks[4], (DEPTH, D), 0.1),
        "w_in": nrm(ks[5], (DEPTH, D, IN_COLS), D ** -0.5),
        "conv_w": nrm(ks[6], (DEPTH, CONV_WIDTH, D_LRU), CONV_WIDTH ** -0.5),
        "conv_b": nrm(ks[7], (DEPTH, D_LRU), 0.02),
        "lru_wa": nrm(ks[8], (DEPTH, LRU_BLOCKS, LRU_BLOCK, LRU_BLOCK), LRU_BLOCK ** -0.5),
        "lru_ba": nrm(ks[9], (DEPTH, D_LRU), 0.02),
        "lru_wx": nrm(ks[10], (DEPTH, LRU_BLOCKS, LRU_BLOCK, LRU_BLOCK), LRU_BLOCK ** -0.5),
        "lru_bx": nrm(ks[11], (DEPTH, D_LRU), 0.02),
        "lru_lambda": lru_lambda,
        "hgrn_lb": nrm(ks[13], (DEPTH + 1, HGRN_HEADS * HGRN_DK), 0.1),
        "hgrn_norm_g": 1.0 + nrm(ks[14], (DEPTH, HGRN_DV), 0.1),
        "w_out": nrm(ks[15], (DEPTH, D_MIX, D), D_MIX ** -0.5),
        "norm2_g": 1.0 + nrm(ks[16], (DEPTH, D), 0.1),
        "w_router": nrm(ks[17], (DEPTH, D, E), D ** -0.5),
        "router_bias": nrm(ks[18], (DEPTH, E), 0.01),
        "w_gate": nrm(ks[19], (DEPTH, E, D, F), D ** -0.5),
        "w_up": nrm(ks[20], (DEPTH, E, D, F), D ** -0.5),
        "w_down": nrm(ks[21], (DEPTH, E, F, D), F ** -0.5),
        "ws_gate": nrm(ks[22], (DEPTH, D, F), D ** -0.5),
        "ws_up": nrm(ks[23], (DEPTH, D, F), D ** -0.5),
        "ws_down": nrm(ks[24], (DEPTH, F, D), F ** -0.5),
        "final_g": 1.0 + nrm(ks[25], (D,), 0.1),
    }


def reference(x, c, w_ada, b_ada, norm1_g, w_in, conv_w, conv_b, lru_wa, lru_ba, lru_wx, lru_bx,
              lru_lambda, hgrn_lb, hgrn_norm_g, w_out, norm2_g, w_router, router_bias,
              w_gate, w_up, w_down, ws_gate, ws_up, ws_down, final_g):
    lower_bounds = jnp.cumsum(jax.nn.softmax(hgrn_lb.astype(jnp.float32), axis=0), axis=0)
    split_at = [int(s) for s in np.cumsum(IN_WIDTHS)[:-1]]
    cond = jax.nn.silu(c)
    for l in range(DEPTH):
        mod = jnp.matmul(cond, w_ada[l]) + b_ada[l]
        sh1, sc1, gt1, sh2, sc2, gt2 = jnp.split(mod[:, None, :], 6, axis=-1)
        h = rms_norm(x, norm1_g[l]) * (1.0 + sc1) + sh1
        proj = jnp.matmul(h, w_in[l])
        u, z, q_in, f_in, v_in, g_in = jnp.split(proj, split_at, axis=-1)
        y_lru = rg_lru_group(u, z, conv_w[l], conv_b[l], lru_wa[l], lru_ba[l],
                             lru_wx[l], lru_bx[l], lru_lambda[l])
        y_hgrn = hgrn2_group(q_in, f_in, v_in, g_in, lower_bounds[l], hgrn_norm_g[l])
        mix = jnp.matmul(jnp.concatenate([y_lru, y_hgrn], axis=-1), w_out[l])
        x = x + gt1 * mix
        h2 = rms_norm(x, norm2_g[l]) * (1.0 + sc2) + sh2
        x = x + gt2 * moe_ffn(h2, w_router[l], router_bias[l], w_gate[l], w_up[l], w_down[l],
                              ws_gate[l], ws_up[l], ws_down[l])
    return rms_norm(x, final_g)
```

```python
import numpy as np
from contextlib import ExitStack
import concourse.bass as bass
import concourse.mybir as mybir
from concourse.bass_utils import run_bass_kernel_spmd

F32 = mybir.dt.float32
BF16 = mybir.dt.bfloat16
I32 = mybir.dt.int32
ALU = mybir.AluOpType
AF = mybir.ActivationFunctionType

D = 2048
NTOK = 2048
TB = 512
NE = 256
EPS = 1e-6
DUMMY = NTOK


class Buf:
    __slots__ = ("name", "w", "r")

    def __init__(self, name):
        self.name = name
        self.w = None
        self.r = []


class Tl:
    def __init__(self, t, name):
        self.t = t
        self.b = Buf(name)

    def __getitem__(self, k):
        return self.t[k]


def _bufs(xs):
    return [x.b if isinstance(x, Tl) else x for x in xs]


class Sched:
    def __init__(self, nc, es, ndma=8):
        self.nc = nc
        self.eng = {"pe": nc.tensor, "act": nc.scalar, "dve": nc.vector, "pool": nc.gpsimd, "sp": nc.sync}
        self.sem = {k: es.enter_context(nc.semaphore("c_" + k)) for k in self.eng}
        self.cnt = {k: 0 for k in self.eng}
        self.seen = {k: {} for k in self.eng}
        nd = {"sp": ndma, "act": ndma, "pool": 16}
        self.dsem = {q: [es.enter_context(nc.semaphore(f"d_{q}{i}")) for i in range(nd[q])] for q in ("sp", "pool", "act")}
        self.dcnt = {q: 0 for q in self.dsem}
        self.dval = {q: [0] * nd[q] for q in self.dsem}
        self.prog = {k: [] for k in self.eng}

    def _waits(self, e, reads, writes):
        need = {}
        for b in reads:
            if b.w is not None:
                s, v = b.w
                if need.get(s, (None, 0))[1] < v:
                    need[s] = (s, v)
        for b in writes:
            if b.w is not None:
                s, v = b.w
                if need.get(s, (None, 0))[1] < v:
                    need[s] = (s, v)
            for (s, v) in b.r:
                if need.get(s, (None, 0))[1] < v:
                    need[s] = (s, v)
        seen = self.seen[e]
        for s, v in need.values():
            if e == "pe" and s is self.sem["pe"]:
                continue
            if seen.get(s, 0) >= v:
                continue
            self.prog[e].append(("w", s, v))
            seen[s] = v

    def _mark(self, ev, reads, writes):
        for b in reads:
            b.r.append(ev)
            if len(b.r) > 16:
                m = {}
                for (s, v) in b.r:
                    if m.get(s, (None, 0))[1] < v:
                        m[s] = (s, v)
                b.r = list(m.values())
        for b in writes:
            b.w = ev
            b.r = []

    def op(self, e, fn, reads=(), writes=()):
        return self.group(e, [fn], reads, writes)

    def group(self, e, fns, reads=(), writes=()):
        reads, writes = _bufs(reads), _bufs(writes)
        self._waits(e, reads, writes)
        self.cnt[e] += 1
        self.prog[e].append(("i", list(fns), self.sem[e], 1))
        ev = (self.sem[e], self.cnt[e])
        self._mark(ev, reads, writes)
        return ev

    def dma(self, q, fn, reads=(), writes=()):
        return self.dma_group(q, [fn], reads, writes)

    def dma_group(self, q, fns, reads=(), writes=()):
        reads, writes = _bufs(reads), _bufs(writes)
        pool = self.dsem[q]
        i = self.dcnt[q] % len(pool)
        self.dcnt[q] += 1
        s = pool[i]
        if self.dval[q][i] > 0 and self.seen[q].get(s, 0) < self.dval[q][i]:
            self.prog[q].append(("w", s, self.dval[q][i]))
            self.seen[q][s] = self.dval[q][i]
        self._waits(q, reads, writes)
        for fn in fns:
            self.prog[q].append(("i", [fn], s, 16))
        self.dval[q][i] += 16 * len(fns)
        ev = (s, self.dval[q][i])
        self._mark(ev, reads, writes)
        return ev

    def finish(self, bufs):
        bufs = _bufs(bufs)
        self._waits("sp", bufs, bufs)

    def emit(self, block):
        def replay(e):
            def run(_eng):
                eng = self.eng[e]
                for it in self.prog[e]:
                    if it[0] == "w":
                        eng.wait_ge(it[1], it[2])
                    else:
                        ins = None
                        for fn in it[1]:
                            ins = fn()
                        ins.then_inc(it[2], it[3])
                self.prog[e] = []
            return run
        block.tensor(replay("pe"))
        block.scalar(replay("act"))
        block.vector(replay("dve"))
        block.gpsimd(replay("pool"))
        block.sync(replay("sp"))


def build_nc(n_pre_blk=4, n_main_blk=4, n_exp=None, dbg=False, ne_alloc=NE):
    nc = bass.Bass("TRN2", target_bir_lowering=False)

    def din(name, shape, dt=F32):
        return nc.dram_tensor(name, list(shape), dt, kind="ExternalInput").ap()

    xT_pre = din("xT_pre", [D, NTOK])
    xT_main = din("xT_main", [D, NTOK])
    x_tok = din("x_tok", [NTOK, D])
    flags = din("flags", [128, 2])
    c_fm = din("c_fm", [128, 16])
    w_ada = din("w_ada", [24, 128, 16, 512])
    bada_fm = din("bada_fm", [128, 32])
    bada_bc = din("bada_bc", [4, 128, D])
    g1_fm = din("g1_fm", [128, 16])
    w_lru = din("w_lru", [8, 128, 16, 256])
    w_hg = din("w_hg", [8, 128, 16, 512])
    lru_vec = din("lru_vec", [128, 9, 8])
    lru_wa = din("lru_wa", [128, 8, 128])
    lru_wx = din("lru_wx", [128, 8, 128])
    hg_lb = din("hg_lb", [128, 2, 8])
    hg_ng = din("hg_ng", [128, 1])
    w_out = din("w_out", [4, 128, 16, 512])
    g2_bc = din("g2_bc", [128, D])
    w_router = din("w_router", [D, NE])
    rb_bc = din("rb_bc", [128, NE])
    w_gate = din("w_gate", [ne_alloc, D, 512])
    w_up = din("w_up", [ne_alloc, D, 512])
    w_down = din("w_down", [ne_alloc, 512, D])
    ws_gate = din("ws_gate", [D, 512])
    ws_up = din("ws_up", [D, 512])
    ws_down = din("ws_down", [512, D])
    fg_bc = din("fg_bc", [128, D])
    out_d = nc.dram_tensor("out", [NTOK, D], F32, kind="ExternalOutput").ap()

    h2_d = nc.dram_tensor("h2_scr", [NTOK + 1, D], BF16).ap()
    x1_d = nc.dram_tensor("x1_scr", [NTOK, D], F32).ap()
    acc_d = nc.dram_tensor("acc_scr", [NTOK + 1, D], F32).ap()
    wd_d = nc.dram_tensor("wd_scr", [NTOK + 1, NE], F32).ap()
    NBLK = 383
    list_d = nc.dram_tensor("list_scr", [128 * 386, 2], F32).ap()
    posm_d = nc.dram_tensor("posm_scr", [NTOK, NE], F32).ap()
    be_d = nc.dram_tensor("be_scr", [384, 2], F32).ap()
    B_posmd, B_bed = Buf("posmd"), Buf("bed")
    gt2_d = nc.dram_tensor("gt2_scr", [128, D], F32).ap()
    B_gt2d = Buf("gt2d")
    B_h2d, B_x1d, B_accd, B_wdd, B_listd, B_out = (Buf(n) for n in ("h2d", "x1d", "accd", "wdd", "listd", "outd"))

    dbg_out = {}
    if dbg:
        for nm, shp in (("dbg_yT", [128, 16 * TB]), ("dbg_x1", [128, D]), ("dbg_h2", [128, D]), ("dbg_wd", [128, NE]),
                        ("dbg_mod", [128, 32]), ("dbg_bc", [128, D]), ("dbg_hT", [128, TB])):
            dbg_out[nm] = nc.dram_tensor(nm, shp, F32, kind="ExternalOutput").ap()

    with ExitStack() as es0:
        S = Sched(nc, es0)

        def sbt(es, name, shape, dt):
            return Tl(es.enter_context(nc.sbuf_tensor("s_" + name, list(shape), dt)), name)

        P = [Tl(es0.enter_context(nc.psum_tensor(f"ps{i}", [128, 512], F32)), f"ps{i}") for i in range(6)]
        PT = Tl(es0.enter_context(nc.psum_tensor("pst", [128, 1024], BF16)), "pst")
        PT2 = Tl(es0.enter_context(nc.psum_tensor("pst2", [128, 1024], BF16)), "pst2")

        ident = sbt(es0, "ident", [128, 128], BF16)
        ones_bf = sbt(es0, "ones_bf", [128, 128], BF16)
        ones_f = sbt(es0, "ones_f", [128, 128], F32)
        flg = sbt(es0, "flg", [128, 2], F32)
        A1 = sbt(es0, "A1", [128, 16], F32)
        B1 = sbt(es0, "B1", [128, 16], F32)
        es01 = ExitStack()
        gt1_bc = sbt(es01, "gt1_bc", [128, D], F32)
        A2_bc = sbt(es01, "A2_bc", [128, D], F32)
        B2_bc = sbt(es01, "B2_bc", [128, D], F32)

        def mm(out, lhsT, rhs, start, stop):
            return lambda: nc.tensor.matmul(out, lhsT=lhsT, rhs=rhs, start=start, stop=stop)

        def tr(out, in_):
            return lambda: nc.tensor.transpose(out=out, in_=in_, identity=ident[:])

        def act(out, in_, func, **kw):
            return lambda: nc.scalar.activation(out=out, in_=in_, func=func, **kw)

        def tt(eng, out, in0, in1, op):
            e = nc.vector if eng == "dve" else nc.gpsimd
            return lambda: e.tensor_tensor(out=out, in0=in0, in1=in1, op=op)

        def ts(eng, out, in0, s1, s2, op0, op1=None):
            e = nc.vector if eng == "dve" else nc.gpsimd
            if op1 is None:
                return lambda: e.tensor_scalar(out=out, in0=in0, scalar1=s1, scalar2=None, op0=op0)
            return lambda: e.tensor_scalar(out=out, in0=in0, scalar1=s1, scalar2=s2, op0=op0, op1=op1)

        def stt(eng, out, in0, scalar, in1, op0, op1):
            e = nc.vector if eng == "dve" else nc.gpsimd
            return lambda: e.scalar_tensor_tensor(out=out, in0=in0, scalar=scalar, in1=in1, op0=op0, op1=op1)

        def cp(eng, out, in_):
            if eng == "act":
                return lambda: nc.scalar.copy(out=out, in_=in_)
            e = nc.vector if eng == "dve" else nc.gpsimd
            return lambda: e.tensor_copy(out=out, in_=in_)

        def dma(q, out, in_):
            e = {"sp": nc.sync, "act": nc.scalar, "pool": nc.gpsimd}[q]
            return lambda: e.dma_start(out=out, in_=in_)

        def dbg_dump(name, tile_ap, reads):
            if dbg:
                S.dma("sp", dma("sp", dbg_out[name], tile_ap), reads=reads, writes=[B_out])

        with ExitStack() as es:
            mod_row = sbt(es, "mod_row", [1, 6 * D], F32)
            cfm = sbt(es, "cfm", [128, 16], F32)
            csg = sbt(es, "csg", [128, 16], F32)
            cond = sbt(es, "cond", [128, 16], BF16)
            wad = [sbt(es, f"wad{i}", [128, 16, 512], BF16) for i in range(2)]
            bfm = sbt(es, "bfm", [128, 32], F32)
            g1t = sbt(es, "g1t", [128, 16], F32)
            modfm = sbt(es, "modfm", [128, 32], F32)
            g2t = sbt(es, "g2t", [128, D], F32)
            gt2_bc = sbt(es, "gt2_bc", [128, D], F32)
            with nc.Block() as block:
                S.op("pool", lambda: nc.gpsimd.memset(ident[:], 0.0), writes=[ident])
                S.op("pool", lambda: nc.gpsimd.affine_select(out=ident[:], in_=ident[:], pattern=[[-1, 128]], compare_op=ALU.not_equal, fill=1.0, base=0, channel_multiplier=1), reads=[ident], writes=[ident])
                S.op("pool", lambda: nc.gpsimd.memset(ones_bf[:], 1.0), writes=[ones_bf])
                S.op("pool", lambda: nc.gpsimd.memset(ones_f[:], 1.0), writes=[ones_f])
                S.dma("sp", dma("sp", flg[:], flags[:, :]), writes=[flg])
                S.dma("sp", dma("sp", cfm[:], c_fm[:, :]), writes=[cfm])
                S.dma("sp", dma("sp", bfm[:], bada_fm[:, :]), writes=[bfm])
                S.dma("sp", dma("sp", g1t[:], g1_fm[:, :]), writes=[g1t])
                S.dma("sp", dma("sp", g2t[:], g2_bc[:, :]), writes=[g2t])
                S.dma("act", dma("act", gt1_bc[:], bada_bc[0]), writes=[gt1_bc])
                S.dma("act", dma("act", B2_bc[:], bada_bc[1]), writes=[B2_bc])
                S.dma("act", dma("act", A2_bc[:], bada_bc[2]), writes=[A2_bc])
                S.dma("act", dma("act", gt2_bc[:], bada_bc[3]), writes=[gt2_bc])
                S.op("act", act(csg[:], cfm[:], AF.Sigmoid), reads=[cfm], writes=[csg])
                S.op("dve", tt("dve", cond[:], cfm[:], csg[:], ALU.mult), reads=[cfm, csg], writes=[cond])
                for nb in range(24):
                    wb = wad[nb % 2]
                    S.dma("pool", dma("pool", wb[:], w_ada[nb]), writes=[wb])
                    pb = P[nb % 2]
                    S.group("pe", [mm(pb[0:1, :], cond[:, kc:kc + 1], wb[:, kc, :], kc == 0, kc == 15) for kc in range(16)],
                            reads=[cond, wb], writes=[pb])
                    S.op("dve", cp("dve", mod_row[0:1, nb * 512:(nb + 1) * 512], pb[0:1, :]), reads=[pb], writes=[mod_row])
                S.group("pe", [mm(P[2][:, j:j + 1], mod_row[0:1, j * 128:(j + 1) * 128], ones_f[0:1, 0:1], True, True) for j in range(32)],
                        reads=[mod_row, ones_f], writes=[P[2]])
                S.op("dve", tt("dve", modfm[:], P[2][:, 0:32], bfm[:], ALU.add), reads=[P[2], bfm], writes=[modfm])
                S.op("dve", cp("dve", B1[:], modfm[:, 0:16]), reads=[modfm], writes=[B1])
                S.op("dve", stt("dve", A1[:], modfm[:, 16:32], 1.0, g1t[:], ALU.add, ALU.mult), reads=[modfm, g1t], writes=[A1])
                dbg_dump("dbg_mod", modfm[:], [modfm])
                for part, dst in ((2, gt1_bc), (3, B2_bc), (4, A2_bc), (5, gt2_bc)):
                    for nb in range(4):
                        pb = P[3 + nb % 2]
                        S.group("pe", [mm(pb[:], ones_f[0:1, :], mod_row[0:1, part * D + nb * 512: part * D + (nb + 1) * 512], True, True)],
                                reads=[mod_row, ones_f], writes=[pb])
                        S.op("dve", tt("dve", dst[:, nb * 512:(nb + 1) * 512], pb[:], dst[:, nb * 512:(nb + 1) * 512], ALU.add), reads=[pb, dst], writes=[dst])
                S.op("dve", stt("dve", A2_bc[:], A2_bc[:], 1.0, g2t[:], ALU.add, ALU.mult), reads=[A2_bc, g2t], writes=[A2_bc])
                dbg_dump("dbg_bc", A2_bc[:], [A2_bc])
                S.dma("sp", dma("sp", gt2_d[:, :], gt2_bc[:]), reads=[gt2_bc], writes=[B_gt2d])
                S.emit(block)

        with ExitStack() as es:
            xbuf = sbt(es, "xbuf", [128, 16, TB], F32)
            hT = sbt(es, "hT", [128, 16, TB], BF16)
            wst = [sbt(es, f"wst{i}", [128, 16, 512], BF16) for i in range(2)]
            yT = [sbt(es, f"yT{c}", [128, TB], BF16) for c in range(16)]
            rstd_bc = sbt(es, "rstd_bc", [128, TB], F32)
            tmpA = sbt(es, "tmpA", [128, TB], F32)
            lvec = sbt(es, "lvec", [128, 9, 8], F32)
            cL = sbt(es, "cL", [128, 8], F32)
            cL2 = sbt(es, "cL2", [128, 8], F32)
            wa_bf = sbt(es, "wa_bf", [128, 8, 128], BF16)
            wx_bf = sbt(es, "wx_bf", [128, 8, 128], BF16)
            hist = sbt(es, "hist", [128, 8, 3], F32)
            hstate = sbt(es, "hstate", [128, 8], F32)
            ubuf = sbt(es, "ubuf", [128, TB + 3], F32)
            xc = sbt(es, "xc", [128, TB], F32)
            xc_bf = sbt(es, "xc_bf", [128, TB], BF16)
            lbt = sbt(es, "lbt", [128, 2, 8], F32)
            hlb = sbt(es, "hlb", [128, 8], F32)
            holb = sbt(es, "holb", [128, 8], F32)
            ngc = sbt(es, "ngc", [128, 1], F32)
            Sst = [sbt(es, f"Sst{h}", [128, 128], F32) for h in range(8)]
            Sbf = [sbt(es, f"Sbf{h}", [128, 128], BF16) for h in range(8)]
            hq = sbt(es, "hq", [128, TB], F32)
            lr = hq
            hf = sbt(es, "hf", [128, TB], F32)
            li = hf
            hlog = sbt(es, "hlog", [128, TB], F32)
            la = hlog
            hk = sbt(es, "hk", [128, TB], F32)
            lm = hk
            bcum = sbt(es, "bcum", [128, TB], F32)
            lb_ = bcum
            refc = sbt(es, "refc", [128, 8], F32)
            nrefc = sbt(es, "nrefc", [128, 8], F32)
            heq = sbt(es, "heq", [128, TB], F32)
            lh = heq
            hek = sbt(es, "hek", [128, TB], F32)
            lz = hek
            qd = sbt(es, "qd", [128, TB], BF16)
            kd = sbt(es, "kd", [128, TB], BF16)
            kdT = sbt(es, "kdT", [64, 8, 128], BF16)
            vtok = sbt(es, "vtok", [64, 8, 128], BF16)
            scT = sbt(es, "scT", [64, 64], BF16)
            maskT = sbt(es, "maskT", [64, 64], F32)
            hsg = sbt(es, "hsg", [128, TB], F32)
            osq = sbt(es, "osq", [128, TB], BF16)
            ho = sbt(es, "ho", [128, TB], F32)
            zeros_c = sbt(es, "zeros_c", [128, 1], F32)
            h2t = sbt(es, "h2t", [128, D], BF16)
            h2T = sbt(es, "h2T", [128, 16, 128], BF16)
            tmpB = sbt(es, "tmpB", [128, D], F32)
            ss = sbt(es, "ss", [128, 2], F32)
            wr_bf = sbt(es, "wr_bf", [128, 16, NE], BF16)
            rbt = sbt(es, "rbt", [128, NE], F32)
            sc = sbt(es, "sc", [128, NE], F32)
            sel = sbt(es, "sel", [128, NE], F32)
            selm = sbt(es, "selm", [128, NE], F32)
            m8 = sbt(es, "m8", [128, 8, 8], F32)
            grp = sbt(es, "grp", [128, 8], F32)
            gm8 = sbt(es, "gm8", [128, 8], F32)
            gmask = sbt(es, "gmask", [128, 8], F32)
            top8 = sbt(es, "top8", [128, 8], F32)
            emask = sbt(es, "emask", [128, NE], F32)
            emask_bf = sbt(es, "emask_bf", [128, NE], BF16)
            wun = sbt(es, "wun", [128, NE], F32)
            wdt = sbt(es, "wdt", [128, NE], F32)
            wsum = sbt(es, "wsum", [128, 2], F32)
            cnt_bc = sbt(es, "cnt_bc", [128, NE], F32)
            ustr = sbt(es, "ustr", [128, 128], BF16)
            val = sbt(es, "val", [128, NE], F32)
            d8 = sbt(es, "d8", [128, 8], F32)
            dest = sbt(es, "dest", [128, 8], I32)
            tokid = sbt(es, "tokid", [128, 16], I32)
            zbuf = sbt(es, "zbuf", [128, TB], F32)
            Ssc = sbt(es, "Ssc", [128, 128], F32)
            tokid_f = sbt(es, "tokid_f", [128, 16], F32)
            recf = sbt(es, "recf", [128, 8, 2], F32)
            zeros8 = sbt(es, "zeros8", [128, 8], F32)
            bcol_i = sbt(es, "bcol_i", [128, 3], I32)
            bcol = sbt(es, "bcol", [128, 3], F32)
            bef = sbt(es, "bef", [128, 3], F32)
            be2 = sbt(es, "be2", [128, 3, 2], F32)
            onesTB = sbt(es, "onesTB", [128, TB], F32)

            with nc.Block() as block:
                S.dma("sp", dma("sp", lvec[:], lru_vec[:, :, :]), writes=[lvec])
                S.dma("pool", dma("pool", wa_bf[:], lru_wa[:, :, :]), writes=[wa_bf])
                S.dma("pool", dma("pool", wx_bf[:], lru_wx[:, :, :]), writes=[wx_bf])
                S.dma("sp", dma("sp", lbt[:], hg_lb[:, :, :]), writes=[lbt])
                S.dma("sp", dma("sp", ngc[:], hg_ng[:, :]), writes=[ngc])
                S.dma("sp", dma("sp", rbt[:], rb_bc[:, :]), writes=[rbt])
                S.dma("pool", dma("pool", wr_bf[:], w_router.rearrange("(kc p) n -> p kc n", p=128)), writes=[wr_bf])
                S.op("act", act(cL[:], lvec[:, 7, :], AF.Exp, scale=-1.0), reads=[lvec], writes=[cL])
                S.op("dve", ts("dve", cL[:], cL[:], 1.0, None, ALU.add), reads=[cL], writes=[cL])
                S.op("act", act(cL[:], cL[:], AF.Ln), reads=[cL], writes=[cL])
                S.op("dve", ts("dve", cL2[:], cL[:], -16.0, None, ALU.mult), reads=[cL], writes=[cL2])
                S.op("dve", ts("dve", cL[:], cL[:], -8.0, None, ALU.mult), reads=[cL], writes=[cL])
                S.op("dve", tt("dve", hlb[:], lbt[:, 0, :], lbt[:, 1, :], ALU.subtract), reads=[lbt], writes=[hlb])
                S.op("act", act(hlb[:], hlb[:], AF.Sigmoid), reads=[hlb], writes=[hlb])
                S.op("dve", ts("dve", holb[:], hlb[:], -1.0, 1.0, ALU.mult, ALU.add), reads=[hlb], writes=[holb])
                S.op("pool", lambda: nc.gpsimd.memset(hist[:], 0.0), writes=[hist])
                S.op("pool", lambda: nc.gpsimd.memset(hstate[:], 0.0), writes=[hstate])
                S.op("pool", lambda: nc.gpsimd.memset(zeros_c[:], 0.0), writes=[zeros_c])
                for h in range(8):
                    S.op("pool", (lambda h=h: nc.gpsimd.memset(Sst[h][:], 0.0)), writes=[Sst[h]])
                    S.op("pool", (lambda h=h: nc.gpsimd.memset(Sbf[h][:], 0.0)), writes=[Sbf[h]])
                S.op("pool", lambda: nc.gpsimd.memset(maskT[:], 1.0), writes=[maskT])
                S.op("pool", lambda: nc.gpsimd.affine_select(out=maskT[:], in_=maskT[:], pattern=[[1, 64]], compare_op=ALU.is_ge, fill=0.0, base=0, channel_multiplier=-1), reads=[maskT], writes=[maskT])
                S.op("pool", lambda: nc.gpsimd.memset(ustr[:], 1.0), writes=[ustr])
                S.op("pool", lambda: nc.gpsimd.affine_select(out=ustr[:], in_=ustr[:], pattern=[[1, 128]], compare_op=ALU.is_ge, fill=0.0, base=-1, channel_multiplier=-1), reads=[ustr], writes=[ustr])
                S.op("pool", lambda: nc.gpsimd.iota(tokid[:], pattern=[[128, 16]], base=0, channel_multiplier=1), writes=[tokid])
                S.op("dve", cp("dve", tokid_f[:], tokid[:]), reads=[tokid], writes=[tokid_f])
                S.op("pool", lambda: nc.gpsimd.memset(cnt_bc[:], 1.0), writes=[cnt_bc])
                S.op("pool", lambda: nc.gpsimd.memset(zeros8[:], 0.0), writes=[zeros8])
                sentv = tmpB[:, 0:772].rearrange("p (n o) -> p n o", o=2)
                S.op("pool", lambda: nc.gpsimd.memset(sentv[:, :, 0:1], float(DUMMY)), writes=[tmpB])
                S.op("pool", lambda: nc.gpsimd.memset(sentv[:, :, 1:2], 0.0), writes=[tmpB])
                S.dma("sp", dma("sp", list_d.rearrange("(p n) o -> p n o", p=128), sentv), reads=[tmpB], writes=[B_listd])
                S.op("pool", lambda: nc.gpsimd.iota(bcol_i[:], pattern=[[128 * 128, 3]], base=0, channel_multiplier=128), writes=[bcol_i])
                S.op("dve", cp("dve", bcol[:], bcol_i[:]), reads=[bcol_i], writes=[bcol])
                S.op("pool", lambda: nc.gpsimd.memset(tmpB[0:1, :], 0.0), writes=[tmpB])
                S.op("pool", lambda: nc.gpsimd.memset(h2t[0:1, :], 0.0), writes=[h2t])
                S.op("pool", lambda: nc.gpsimd.memset(onesTB[:], 1.0), writes=[onesTB])
                S.dma("sp", dma("sp", h2_d[NTOK:NTOK + 1, :], h2t[0:1, :]), reads=[h2t], writes=[B_h2d])
                S.dma("sp", dma("sp", acc_d[NTOK:NTOK + 1, :], tmpB[0:1, :]), reads=[tmpB], writes=[B_accd])
                S.dma("sp", dma("sp", wd_d[NTOK:NTOK + 1, :], tmpB[0:1, 0:NE]), reads=[tmpB], writes=[B_wdd])

                def mixer_block(xT_src, blk, main, first_of_seq, first_of_main, tile_base, last_of_pre=False):
                    t0 = blk * TB
                    src_v = xT_src.rearrange("(kc p) t -> p kc t", p=128)
                    S.dma("sp", dma("sp", xbuf[:, 0:8, :], src_v[:, 0:8, t0:t0 + TB]), writes=[xbuf])
                    S.dma("act", dma("act", xbuf[:, 8:16, :], src_v[:, 8:16, t0:t0 + TB]), writes=[xbuf])
                    for kc in range(16):
                        S.op("act", act(hT[:, kc, :], xbuf[:, kc, :], AF.Square), reads=[xbuf], writes=[hT])
                    S.group("pe", [mm(P[5][:], ones_bf[:], hT[:, kc, :], kc == 0, kc == 15) for kc in range(16)], reads=[ones_bf, hT], writes=[P[5]])
                    S.op("act", act(rstd_bc[:], P[5][:], AF.Sqrt, scale=1.0 / D, bias=eps_c[:, 0:1]), reads=[P[5], eps_c], writes=[rstd_bc])
                    S.op("dve", lambda: nc.vector.reciprocal(out=rstd_bc[:], in_=rstd_bc[:]), reads=[rstd_bc], writes=[rstd_bc])
                    for kc in range(16):
                        S.op("dve", tt("dve", tmpA[:], xbuf[:, kc, :], rstd_bc[:], ALU.mult), reads=[xbuf, rstd_bc], writes=[tmpA])
                        S.op("act", act(hT[:, kc, :], tmpA[:], AF.Identity, scale=A1[:, kc:kc + 1], bias=B1[:, kc:kc + 1]), reads=[tmpA, A1, B1], writes=[hT])
                    def lru_inproj(cb):
                        wb = wst[cb % 2]
                        ncols = 256 if main else 128
                        S.dma("pool", dma("pool", wb[:, :, 0:ncols], w_lru[cb, :, :, 0:ncols]), writes=[wb])
                        S.group("pe", [mm(P[0][:], wb[:, kc, 0:128], hT[:, kc, :], kc == 0, kc == 15) for kc in range(16)], reads=[wb, hT], writes=[P[0]])
                        if main:
                            S.group("pe", [mm(P[1][:], wb[:, kc, 128:256], hT[:, kc, :], kc == 0, kc == 15) for kc in range(16)], reads=[wb, hT], writes=[P[1]])

                    def lru_evac(cb):
                        if first_of_main:
                            S.op("dve", ts("dve", hist[:, cb, :], hist[:, cb, :], flg[:, 0:1], None, ALU.mult), reads=[hist, flg], writes=[hist])
                            S.op("dve", ts("dve", hstate[:, cb:cb + 1], hstate[:, cb:cb + 1], flg[:, 0:1], None, ALU.mult), reads=[hstate, flg], writes=[hstate])
                        S.op("dve", cp("dve", ubuf[:, 0:3], hist[:, cb, :]), reads=[hist], writes=[ubuf])
                        S.op("act", cp("act", ubuf[:, 3:TB + 3], P[0][:]), reads=[P[0]], writes=[ubuf])
                        if main:
                            S.op("act", cp("act", zbuf[:], P[1][:]), reads=[P[1]], writes=[zbuf])
                        S.op("dve", cp("dve", hist[:, cb, :], ubuf[:, TB:TB + 3]), reads=[ubuf], writes=[hist])

                    def lru_rest(cb):
                        S.op("dve", ts("dve", xc[:], ubuf[:, 3:TB + 3], lvec[:, 3, cb:cb + 1], lvec[:, 4, cb:cb + 1], ALU.mult, ALU.add), reads=[ubuf, lvec], writes=[xc])
                        for j in range(3):
                            S.op("dve", stt("dve", xc[:], ubuf[:, j:j + TB], lvec[:, j, cb:cb + 1], xc[:], ALU.mult, ALU.add), reads=[ubuf, lvec, xc], writes=[xc])
                        S.op("act", cp("act", xc_bf[:], xc[:]), reads=[xc], writes=[xc_bf])
                        S.group("pe", [mm(P[2][:], wa_bf[:, cb, :], xc_bf[:], True, True)], reads=[wa_bf, xc_bf], writes=[P[2]])
                        S.group("pe", [mm(P[3][:], wx_bf[:, cb, :], xc_bf[:], True, True)], reads=[wx_bf, xc_bf], writes=[P[3]])
                        S.op("act", act(lr[:], P[2][:], AF.Sigmoid, bias=lvec[:, 5, cb:cb + 1]), reads=[P[2], lvec], writes=[lr])
                        S.op("act", act(li[:], P[3][:], AF.Sigmoid, bias=lvec[:, 6, cb:cb + 1]), reads=[P[3], lvec], writes=[li])
                        S.op("act", act(la[:], lr[:], AF.Exp, scale=cL[:, cb:cb + 1]), reads=[lr, cL], writes=[la])
                        S.op("act", act(lm[:], lr[:], AF.Exp, scale=cL2[:, cb:cb + 1]), reads=[lr, cL2], writes=[lm])
                        S.op("dve", ts("dve", lm[:], lm[:], -1.0, 1.0, ALU.mult, ALU.add), reads=[lm], writes=[lm])
                        S.op("act", act(lm[:], lm[:], AF.Sqrt), reads=[lm], writes=[lm])
                        if first_of_seq:
                            S.op("pool", lambda: nc.gpsimd.memset(lm[:, 0:1], 1.0), reads=[lm], writes=[lm])
                        elif first_of_main:
                            S.op("dve", ts("dve", lm[:, 0:1], lm[:, 0:1], flg[:, 0:1], flg[:, 1:2], ALU.mult, ALU.add), reads=[lm, flg], writes=[lm])
                        S.op("dve", tt("dve", lb_[:], lm[:], li[:], ALU.mult), reads=[lm, li], writes=[lb_])
                        S.op("dve", tt("dve", lb_[:], lb_[:], xc[:], ALU.mult), reads=[lb_, xc], writes=[lb_])
                        S.op("dve", (lambda cb=cb: nc.vector.tensor_tensor_scan(out=lh[:], data0=la[:], data1=lb_[:], initial=hstate[:, cb:cb + 1], op0=ALU.mult, op1=ALU.add)),
                             reads=[la, lb_, hstate], writes=[lh])
                        S.op("dve", cp("dve", hstate[:, cb:cb + 1], lh[:, TB - 1:TB]), reads=[lh], writes=[hstate])
                        if main:
                            S.op("act", act(lz[:], zbuf[:], AF.Square), reads=[zbuf], writes=[lz])
                            S.op("dve", ts("dve", lz[:], lz[:], 0.044715, 1.0, ALU.mult, ALU.add), reads=[lz], writes=[lz])
                            S.op("dve", tt("dve", lz[:], lz[:], zbuf[:], ALU.mult), reads=[lz, zbuf], writes=[lz])
                            S.op("act", act(lz[:], lz[:], AF.Sigmoid, scale=1.5957691216057308), reads=[lz], writes=[lz])
                            S.op("dve", tt("dve", lz[:], lz[:], zbuf[:], ALU.mult), reads=[lz, zbuf], writes=[lz])
                            S.op("dve", tt("dve", yT[cb][:], lh[:], lz[:], ALU.mult), reads=[lh, lz], writes=[yT[cb]])

                    lru_inproj(0)
                    for cb in range(8):
                        lru_evac(cb)
                        if cb + 1 < 8:
                            lru_inproj(cb + 1)
                        lru_rest(cb)
                    for hd in range(8):
                        wb = wst[hd % 2]
                        if main:
                            S.dma("pool", dma("pool", wb[:], w_hg[hd, :, :, :]), writes=[wb])
                        else:
                            S.dma("pool", dma("pool", wb[:, :, 128:384], w_hg[hd, :, :, 128:384]), writes=[wb])
                        S.group("pe", [mm(P[1][:], wb[:, kc, 128:256], hT[:, kc, :], kc == 0, kc == 15) for kc in range(16)], reads=[wb, hT], writes=[P[1]])
                        if main:
                            S.group("pe", [mm(P[0][:], wb[:, kc, 0:128], hT[:, kc, :], kc == 0, kc == 15) for kc in range(16)], reads=[wb, hT], writes=[P[0]])
                            S.group("pe", [mm(P[2][:], wb[:, kc, 384:512], hT[:, kc, :], kc == 0, kc == 15) for kc in range(16)], reads=[wb, hT], writes=[P[2]])
                        for half in range(2):
                            fns = []
                            for c4 in range(4):
                                c = half * 4 + c4
                                for kc in range(16):
                                    fns.append(mm(P[3][0:64, c4 * 128:(c4 + 1) * 128], hT[:, kc, c * 64:(c + 1) * 64], wb[:, kc, 256:384], kc == 0, kc == 15))
                            S.group("pe", fns, reads=[wb, hT], writes=[P[3]])
                            S.op("act", cp("act", vtok[:, half * 4:(half + 1) * 4, :], P[3][0:64, :].rearrange("p (c v) -> p c v", c=4)), reads=[P[3]], writes=[vtok])
                        S.op("act", act(hf[:], P[1][:], AF.Sigmoid), reads=[P[1]], writes=[hf])
                        S.op("dve", ts("dve", hf[:], hf[:], holb[:, hd:hd + 1], hlb[:, hd:hd + 1], ALU.mult, ALU.add), reads=[hf, holb, hlb], writes=[hf])
                        S.op("act", act(hlog[:], hf[:], AF.Ln), reads=[hf], writes=[hlog])
                        S.op("pool", ts("pool", hk[:], hf[:], -1.0, 1.0, ALU.mult, ALU.add), reads=[hf], writes=[hk])
                        S.op("dve", lambda: nc.vector.tensor_tensor_scan(out=bcum[:], data0=onesTB[:], data1=hlog[:], initial=0.0, op0=ALU.mult, op1=ALU.add),
                             reads=[hlog, onesTB], writes=[bcum])
                        S.op("dve", cp("dve", refc[:, 0:1], zeros_c[:]), reads=[zeros_c], writes=[refc])
                        S.op("dve", cp("dve", refc[:, 1:8], bcum[:, 63:TB - 1:64]), reads=[bcum], writes=[refc])
                        S.op("dve", ts("dve", nrefc[:], refc[:], -1.0, None, ALU.mult), reads=[refc], writes=[nrefc])
                        for c in range(8):
                            sl = slice(c * 64, (c + 1) * 64)
                            S.op("act", act(heq[:, sl], bcum[:, sl], AF.Exp, bias=nrefc[:, c:c + 1]), reads=[bcum, nrefc], writes=[heq])
                            S.op("act", act(hek[:, sl], bcum[:, sl], AF.Exp, scale=-1.0, bias=refc[:, c:c + 1]), reads=[bcum, refc], writes=[hek])
                        S.op("pool", tt("pool", kd[:], hk[:], hek[:], ALU.mult), reads=[hk, hek], writes=[kd])
                        if main:
                            S.op("act", act(hq[:], P[0][:], AF.Silu), reads=[P[0]], writes=[hq])
                            S.op("dve", tt("dve", qd[:], hq[:], heq[:], ALU.mult), reads=[hq, heq], writes=[qd])
                            S.op("act", act(hsg[:], P[2][:], AF.Silu), reads=[P[2]], writes=[hsg])
                        S.group("pe", [tr(PT[0:64, c * 128:(c + 1) * 128], kd[:, c * 64:(c + 1) * 64]) for c in range(8)], reads=[kd, ident], writes=[PT])
                        S.op("dve", cp("dve", kdT[:], PT[0:64, :].rearrange("p (c k) -> p c k", c=8)), reads=[PT], writes=[kdT])
                        for c in range(8):
                            sl = slice(c * 64, (c + 1) * 64)
                            if main:
                                S.group("pe", [mm(P[4][0:64, 0:64], kd[:, sl], qd[:, sl], True, True)], reads=[kd, qd], writes=[P[4]])
                                S.op("dve", tt("dve", scT[:], P[4][0:64, 0:64], maskT[:], ALU.mult), reads=[P[4], maskT], writes=[scT])
                                S.group("pe", [mm(P[0][:, sl], vtok[:, c, :], scT[:], True, False),
                                               mm(P[0][:, sl], Sbf[hd][:], qd[:, sl], False, True)], reads=[vtok, scT, Sbf[hd], qd], writes=[P[0]])
                            dS = heq[:, c * 64 + 63:c * 64 + 64]
                            S.op("dve", ts("dve", Ssc[:], Sst[hd][:], dS, None, ALU.mult), reads=[Sst[hd], heq], writes=[Ssc])
                            S.group("pe", [mm(P[5][:, 0:128], kdT[:, c, :], vtok[:, c, :], True, True)], reads=[kdT, vtok], writes=[P[5]])
                            S.op("dve", stt("dve", Sbf[hd][:], P[5][:, 0:128], dS, Ssc[:], ALU.mult, ALU.add), reads=[P[5], heq, Ssc], writes=[Sbf[hd]])
                            S.op("dve", stt("dve", Sst[hd][:], P[5][:, 0:128], dS, Ssc[:], ALU.mult, ALU.add), reads=[P[5], heq, Ssc], writes=[Sst[hd]])
                        if main:
                            S.op("act", act(osq[:], P[0][:], AF.Square), reads=[P[0]], writes=[osq])
                            S.group("pe", [mm(P[1][:], ones_bf[:], osq[:], True, True)], reads=[ones_bf, osq], writes=[P[1]])
                            S.op("act", act(ho[:], P[1][:], AF.Sqrt, scale=1.0 / 128, bias=eps_c[:, 0:1]), reads=[P[1], eps_c], writes=[ho])
                            S.op("dve", lambda: nc.vector.reciprocal(out=ho[:], in_=ho[:]), reads=[ho], writes=[ho])
                            S.op("dve", tt("dve", ho[:], P[0][:], ho[:], ALU.mult), reads=[P[0], ho], writes=[ho])
                            S.op("dve", stt("dve", yT[8 + hd][:], ho[:], ngc[:, 0:1], hsg[:], ALU.mult, ALU.mult), reads=[ho, ngc, hsg], writes=[yT[8 + hd]])
                        if last_of_pre:
                            S.op("dve", ts("dve", Sst[hd][:], Sst[hd][:], flg[:, 0:1], None, ALU.mult), reads=[Sst[hd], flg], writes=[Sst[hd]])
                            S.op("dve", cp("dve", Sbf[hd][:], Sst[hd][:]), reads=[Sst[hd]], writes=[Sbf[hd]])
                    if not main:
                        return
                    if dbg and blk == 0:
                        for c in range(16):
                            S.op("dve", cp("dve", tmpB[:, 0:TB], yT[c][:]), reads=[yT[c]], writes=[tmpB])
                            S.dma("sp", dma("sp", dbg_out["dbg_yT"][:, c * TB:(c + 1) * TB], tmpB[:, 0:TB]), reads=[tmpB], writes=[B_out])
                    xv = xbuf[:].rearrange("p a b -> p (a b)").rearrange("p (t d) -> p t d", t=4)
                    for ti in range(4):
                        q = "sp" if ti % 2 == 0 else "act"
                        S.dma(q, dma(q, xv[:, ti, :], x_tok[t0 + ti * 128:t0 + (ti + 1) * 128, :]), writes=[xbuf])
                    for nb in range(4):
                        wb = wst[nb % 2]
                        S.dma("pool", dma("pool", wb[:], w_out[nb]), writes=[wb])
                        for ti in range(4):
                            pb = P[ti]
                            S.group("pe", [mm(pb[:], yT[c][:, ti * 128:(ti + 1) * 128], wb[:, c, :], c == 0, c == 15) for c in range(16)], reads=yT + [wb], writes=[pb])
                            S.op("dve", tt("dve", tmpA[:], pb[:], gt1_bc[:, nb * 512:(nb + 1) * 512], ALU.mult), reads=[pb, gt1_bc], writes=[tmpA])
                            S.op("pool", tt("pool", xv[:, ti, nb * 512:(nb + 1) * 512], xv[:, ti, nb * 512:(nb + 1) * 512], tmpA[:], ALU.add), reads=[xbuf, tmpA], writes=[xbuf])
                    for ti in range(4):
                        gt = tile_base + ti
                        r0 = gt * 128
                        S.dma("sp", dma("sp", x1_d[r0:r0 + 128, :], xv[:, ti, :]), reads=[xbuf], writes=[B_x1d])
                        S.op("act", act(tmpB[:], xv[:, ti, :], AF.Square, accum_out=ss[:, 0:1]), reads=[xbuf], writes=[tmpB, ss])
                        S.op("act", act(ss[:, 1:2], ss[:, 0:1], AF.Sqrt, scale=1.0 / D, bias=eps_c[:, 0:1]), reads=[ss, eps_c], writes=[ss])
                        S.op("dve", lambda: nc.vector.reciprocal(out=ss[:, 1:2], in_=ss[:, 1:2]), reads=[ss], writes=[ss])
                        S.op("dve", stt("dve", tmpB[:], xv[:, ti, :], ss[:, 1:2], A2_bc[:], ALU.mult, ALU.mult), reads=[xbuf, ss, A2_bc], writes=[tmpB])
                        S.op("pool", tt("pool", h2t[:], tmpB[:], B2_bc[:], ALU.add), reads=[tmpB, B2_bc], writes=[h2t])
                        S.dma("act", dma("act", h2_d[r0:r0 + 128, :], h2t[:]), reads=[h2t], writes=[B_h2d])
                        if dbg and gt == 0:
                            S.dma("sp", dma("sp", dbg_out["dbg_x1"], xv[:, ti, :]), reads=[xbuf], writes=[B_out])
                            S.op("dve", cp("dve", tmpB[:], h2t[:]), reads=[h2t], writes=[tmpB])
                            S.dma("sp", dma("sp", dbg_out["dbg_h2"], tmpB[:]), reads=[tmpB], writes=[B_out])
                        for hh in range(2):
                            S.group("pe", [tr(PT[:, k8 * 128:(k8 + 1) * 128], h2t[:, (hh * 8 + k8) * 128:(hh * 8 + k8 + 1) * 128]) for k8 in range(8)], reads=[h2t, ident], writes=[PT])
                            S.op("act" if hh == 0 else "dve", cp("act" if hh == 0 else "dve", h2T[:, hh * 8:(hh + 1) * 8, :], PT[:].rearrange("p (k t) -> p k t", k=8)), reads=[PT], writes=[h2T])
                        S.group("pe", [mm(P[4][:, 0:NE], h2T[:, kc, :], wr_bf[:, kc, :], kc == 0, kc == 15) for kc in range(16)], reads=[h2T, wr_bf], writes=[P[4]])
                        S.op("act", act(sc[:], P[4][:, 0:NE], AF.Sigmoid), reads=[P[4]], writes=[sc])
                        S.op("dve", tt("dve", sel[:], sc[:], rbt[:], ALU.add), reads=[sc, rbt], writes=[sel])
                        for g in range(8):
                            S.op("dve", (lambda g=g: nc.vector.max(out=m8[:, g, :], in_=sel[:, g * 32:(g + 1) * 32])), reads=[sel], writes=[m8])
                        S.op("dve", tt("dve", grp[:], m8[:, :, 0], m8[:, :, 1], ALU.add), reads=[m8], writes=[grp])
                        S.op("dve", lambda: nc.vector.max(out=gm8[:], in_=grp[:]), reads=[grp], writes=[gm8])
                        S.op("dve", ts("dve", gmask[:], grp[:], gm8[:, 3:4], None, ALU.is_ge), reads=[grp, gm8], writes=[gmask])
                        for g in range(8):
                            S.op("dve", ts("dve", selm[:, g * 32:(g + 1) * 32], sel[:, g * 32:(g + 1) * 32], 10.0, gmask[:, g:g + 1], ALU.add, ALU.mult), reads=[sel, gmask], writes=[selm])
                        S.op("dve", lambda: nc.vector.max(out=top8[:], in_=selm[:]), reads=[selm], writes=[top8])
                        S.op("dve", ts("dve", emask[:], selm[:], top8[:, 7:8], None, ALU.is_ge), reads=[selm, top8], writes=[emask])
                        S.op("pool", cp("pool", emask_bf[:], emask[:]), reads=[emask], writes=[emask_bf])
                        S.op("dve", tt("dve", wun[:], sc[:], emask[:], ALU.mult), reads=[sc, emask], writes=[wun])
                        S.op("dve", lambda: nc.vector.reduce_sum(out=wsum[:, 0:1], in_=wun[:], axis=mybir.AxisListType.X), reads=[wun], writes=[wsum])
                        S.op("dve", lambda: nc.vector.reciprocal(out=wsum[:, 1:2], in_=wsum[:, 0:1]), reads=[wsum], writes=[wsum])
                        S.op("dve", ts("dve", wdt[:], wun[:], wsum[:, 1:2], 2.5, ALU.mult, ALU.mult), reads=[wun, wsum], writes=[wdt])
                        S.dma("sp", dma("sp", wd_d[r0:r0 + 128, :], wdt[:]), reads=[wdt], writes=[B_wdd])
                        if dbg and gt == 0:
                            S.dma("sp", dma("sp", dbg_out["dbg_wd"], wdt[:]), reads=[wdt], writes=[B_out])
                        S.group("pe", [mm(P[5][:, 0:NE], ustr[:], emask_bf[:], True, True), mm(P[5][:, NE:2 * NE], ones_bf[:], emask_bf[:], True, True)],
                                reads=[ustr, ones_bf, emask_bf], writes=[P[5]])
                        S.op("dve", tt("dve", val[:], P[5][:, 0:NE], cnt_bc[:], ALU.add), reads=[P[5], cnt_bc], writes=[val])
                        S.op("dve", tt("dve", val[:], val[:], emask[:], ALU.mult), reads=[val, emask], writes=[val])
                        S.op("dve", tt("dve", cnt_bc[:], P[5][:, NE:2 * NE], cnt_bc[:], ALU.add), reads=[P[5], cnt_bc], writes=[cnt_bc])
                        S.dma("act", dma("act", posm_d[r0:r0 + 128, :], val[:]), reads=[val], writes=[B_posmd])

                def dispatch_pass2():
                    S.op("pool", lambda: nc.gpsimd.memset(sel[:], 0.0), writes=[sel])
                    for m in range(16):
                        S.op("dve", stt("dve", sel[:], cnt_bc[:], 1.0 + 128.0 * m, sel[:], ALU.is_gt, ALU.add), reads=[cnt_bc, sel], writes=[sel])
                    S.op("dve", ts("dve", sel[:], sel[:], 128.0, None, ALU.mult), reads=[sel], writes=[sel])
                    S.op("dve", lambda: nc.vector.tensor_tensor_scan(out=selm[:], data0=onesTB[:, 0:NE], data1=sel[:], initial=0.0, op0=ALU.mult, op1=ALU.add), reads=[onesTB, sel], writes=[selm])
                    S.op("dve", tt("dve", wun[:], selm[:], sel[:], ALU.subtract), reads=[selm, sel], writes=[wun])
                    for j in range(3):
                        S.op("dve", ts("dve", emask[:], selm[:], bcol[:, j:j + 1], None, ALU.is_le), reads=[selm, bcol], writes=[emask])
                        S.op("dve", (lambda j=j: nc.vector.reduce_sum(out=bef[:, j:j + 1], in_=emask[:], axis=mybir.AxisListType.X)), reads=[emask], writes=[bef])
                    S.op("dve", ts("dve", be2[:, :, 0], bef[:], 2048.0, None, ALU.mult), reads=[bef], writes=[be2])
                    S.op("dve", ts("dve", be2[:, :, 1], bef[:], 512.0, None, ALU.mult), reads=[bef], writes=[be2])
                    for j in range(3):
                        S.dma("sp", dma("sp", be_d[j * 128:(j + 1) * 128, :], be2[:, j, :]), reads=[be2], writes=[B_bed])
                    for t in range(4 * n_main_blk):
                        r0 = t * 128
                        S.dma("sp", dma("sp", val[:], posm_d[r0:r0 + 128, :]), reads=[B_posmd], writes=[val])
                        S.dma("act", dma("act", wdt[:], wd_d[r0:r0 + 128, :]), reads=[B_wdd], writes=[wdt])
                        S.op("dve", ts("dve", emask[:], val[:], 0.0, None, ALU.is_gt), reads=[val], writes=[emask])
                        S.op("dve", tt("dve", val[:], val[:], wun[:], ALU.add), reads=[val, wun], writes=[val])
                        S.op("dve", tt("dve", val[:], val[:], emask[:], ALU.mult), reads=[val, emask], writes=[val])
                        S.op("dve", lambda: nc.vector.max(out=d8[:], in_=val[:]), reads=[val], writes=[d8])
                        S.op("dve", ts("dve", dest[:], d8[:], -1.0, None, ALU.add), reads=[d8], writes=[dest])
                        S.op("dve", (lambda t=t: nc.vector.tensor_scalar(out=recf[:, :, 0], in0=zeros8[:], scalar1=tokid_f[:, t:t + 1], scalar2=None, op0=ALU.add)), reads=[zeros8, tokid_f], writes=[recf])
                        for k in range(8):
                            S.op("dve", stt("dve", sc[:], val[:], d8[:, k:k + 1], wdt[:], ALU.is_equal, ALU.mult), reads=[val, d8, wdt], writes=[sc])
                            S.op("dve", (lambda k=k: nc.vector.reduce_sum(out=recf[:, k, 1:2], in_=sc[:], axis=mybir.AxisListType.X)), reads=[sc], writes=[recf])
                        S.dma_group("pool", [(lambda k=k: nc.gpsimd.indirect_dma_start(out=list_d[:, :], out_offset=bass.IndirectOffsetOnAxis(ap=dest[:, k:k + 1], axis=0),
                                                                                      in_=recf[:, k, :], in_offset=None)) for k in range(8)],
                                    reads=[dest, recf], writes=[B_listd])

                eps_c = sbt(es, "eps_c", [128, 1], F32)
                S.op("pool", lambda: nc.gpsimd.memset(eps_c[:], EPS), writes=[eps_c])
                for blk in range(n_pre_blk):
                    mixer_block(xT_pre, blk + (4 - n_pre_blk), False, blk == 0, False, 0, last_of_pre=(blk == n_pre_blk - 1))
                for blk in range(n_main_blk):
                    mixer_block(xT_main, blk, True, False, blk == 0, blk * 4)
                dispatch_pass2()
                S.emit(block)
        es01.close()

        with ExitStack() as es:
            Wg = [sbt(es, f"Wg{i}", [128, 16, 512], BF16) for i in range(3)]
            Wu = [sbt(es, f"Wu{i}", [128, 16, 512], BF16) for i in range(3)]
            Wd = [sbt(es, f"Wd{i}", [128, 4, D], BF16) for i in range(3)]
            X = [sbt(es, f"X{i}", [128, D], BF16) for i in range(3)]
            XT = [sbt(es, f"XT{i}", [128, 16, 128], BF16) for i in range(2)]
            idx = [sbt(es, f"idx{i}", [128, 1], I32) for i in range(3)]
            rec = [sbt(es, f"rec{i}", [128, 2], F32) for i in range(3)]
            ebc = [sbt(es, f"ebc{i}", [128, 2], F32) for i in range(3)]
            widx_g = [sbt(es, f"widx_g{i}", [128, 1], I32) for i in range(3)]
            widx_d = [sbt(es, f"widx_d{i}", [128, 1], I32) for i in range(3)]
            iog_i = sbt(es, "iog_i", [128, 2], I32)
            iog_f = sbt(es, "iog_f", [128, 2], F32)
            sgt = [sbt(es, f"sgt{i}", [128, 512], F32) for i in range(2)]
            at = [sbt(es, f"at{i}", [128, 512], BF16) for i in range(2)]
            aT = [sbt(es, f"aT{i}", [128, 4, 128], BF16) for i in range(2)]
            yb = [sbt(es, f"yb{i}", [128, D], F32) for i in range(2)]
            one_c = sbt(es, "one_c", [128, 1], F32)
            eps2 = sbt(es, "eps2", [128, 1], F32)
            fgt = sbt(es, "fgt", [128, D], F32)
            xa, xb = yb[0], yb[1]
            ss2 = sbt(es, "ss2", [128, 2], F32)
            gt2_bc = sbt(es, "gt2_bc2", [128, D], F32)
            with nc.Block() as block:
                S.op("pool", lambda: nc.gpsimd.memset(one_c[:], 1.0), writes=[one_c])
                S.op("pool", lambda: nc.gpsimd.memset(eps2[:], EPS), writes=[eps2])
                for i in range(3):
                    S.op("pool", (lambda i=i: nc.gpsimd.memset(X[i][:], 0.0)), writes=[X[i]])
                    S.op("pool", (lambda i=i: nc.gpsimd.memset(Wg[i][:], 0.0)), writes=[Wg[i]])
                    S.op("pool", (lambda i=i: nc.gpsimd.memset(Wu[i][:], 0.0)), writes=[Wu[i]])
                    S.op("pool", (lambda i=i: nc.gpsimd.memset(Wd[i][:], 0.0)), writes=[Wd[i]])
                S.op("pool", lambda: nc.gpsimd.iota(iog_i[:, 0:1], pattern=[[0, 1]], base=0, channel_multiplier=16), writes=[iog_i])
                S.op("pool", lambda: nc.gpsimd.iota(iog_i[:, 1:2], pattern=[[0, 1]], base=0, channel_multiplier=4), reads=[iog_i], writes=[iog_i])
                S.op("dve", cp("dve", iog_f[:], iog_i[:]), reads=[iog_i], writes=[iog_f])
                n_sh = 4 * n_main_blk
                bregs = {}
                n_blk = NBLK if n_exp is None else n_exp
                order = []
                lo, hi = 0, n_blk - 1
                while lo <= hi:
                    order.append(lo)
                    if hi != lo:
                        order.append(hi)
                    lo += 1
                    hi -= 1
                units = [("s", t) for t in range(n_sh)] + [("e", e) for e in order]
                wg_flat = w_gate.rearrange("e d f -> (e d) f")
                wu_flat = w_up.rearrange("e d f -> (e d) f")
                wd_flat = w_down.rearrange("e f d -> (e f) d")

                def wslot(ui):
                    return 0 if units[ui][0] == "s" else (ui - n_sh + 1) % 3

                def load(ui):
                    kind, k = units[ui]
                    sl = ui % 3
                    ws = wslot(ui)
                    if kind == "s":
                        if k == 0:
                            S.dma("pool", dma("pool", Wg[0][:], ws_gate.rearrange("(p kc) f -> p kc f", p=128)), writes=[Wg[0]])
                            S.dma("pool", dma("pool", Wu[0][:], ws_up.rearrange("(p kc) f -> p kc f", p=128)), writes=[Wu[0]])
                            S.dma("pool", dma("pool", Wd[0][:], ws_down.rearrange("(p fc) d -> p fc d", p=128)), writes=[Wd[0]])
                        S.dma("sp", dma("sp", X[sl][:], h2_d[k * 128:(k + 1) * 128, :]), reads=[B_h2d], writes=[X[sl]])
                    else:
                        S.dma("sp", dma("sp", ebc[ws][:], be_d[k:k + 1, :].partition_broadcast(128)), reads=[B_bed], writes=[ebc[ws]])
                        S.op("dve", ts("dve", widx_g[ws][:], iog_f[:, 0:1], ebc[ws][:, 0:1], None, ALU.add), reads=[iog_f, ebc[ws]], writes=[widx_g[ws]])
                        S.op("dve", ts("dve", widx_d[ws][:], iog_f[:, 1:2], ebc[ws][:, 1:2], None, ALU.add), reads=[iog_f, ebc[ws]], writes=[widx_d[ws]])

                        def wgather(dst, flat, wi, nrow):
                            def f():
                                if nrow not in bregs:
                                    bregs[nrow] = nc.gpsimd.to_reg(nrow - 1)
                                return nc.gpsimd.indirect_dma_start(out=dst, out_offset=None, in_=flat[:, :], in_offset=bass.IndirectOffsetOnAxis(ap=wi, axis=0),
                                                                    bounds_check=bregs[nrow], oob_is_err=False)
                            return f
                        S.dma("pool", wgather(Wg[ws][:].rearrange("p a b -> p (a b)"), wg_flat, widx_g[ws][:, 0:1], ne_alloc * D), reads=[widx_g[ws]], writes=[Wg[ws]])
                        S.dma("pool", wgather(Wu[ws][:].rearrange("p a b -> p (a b)"), wu_flat, widx_g[ws][:, 0:1], ne_alloc * D), reads=[widx_g[ws]], writes=[Wu[ws]])
                        S.dma("pool", wgather(Wd[ws][:].rearrange("p a b -> p (a b)"), wd_flat, widx_d[ws][:, 0:1], ne_alloc * 512), reads=[widx_d[ws]], writes=[Wd[ws]])
                        S.dma("sp", dma("sp", rec[sl][:], list_d[k * 128:(k + 1) * 128, :]), reads=[B_listd], writes=[rec[sl]])
                        S.op("dve", cp("dve", idx[sl][:], rec[sl][:, 0:1]), reads=[rec[sl]], writes=[idx[sl]])
                        S.dma("pool", (lambda sl=sl: nc.gpsimd.indirect_dma_start(out=X[sl][:], out_offset=None, in_=h2_d[:, :], in_offset=bass.IndirectOffsetOnAxis(ap=idx[sl][:, 0:1], axis=0))),
                              reads=[idx[sl], B_h2d], writes=[X[sl]])

                def stage_a(ui):
                    sl = ui % 3
                    xt = XT[ui % 2]
                    S.group("pe", [tr(PT[:, k8 * 128:(k8 + 1) * 128], X[sl][:, k8:D:16]) for k8 in range(8)], reads=[X[sl], ident], writes=[PT])
                    S.group("pe", [tr(PT2[:, k8 * 128:(k8 + 1) * 128], X[sl][:, (8 + k8):D:16]) for k8 in range(8)], reads=[X[sl], ident], writes=[PT2])
                    S.op("act", cp("act", xt[:, 0:8, :], PT[:].rearrange("p (k t) -> p k t", k=8)), reads=[PT], writes=[xt])
                    S.op("dve", cp("dve", xt[:, 8:16, :], PT2[:].rearrange("p (k t) -> p k t", k=8)), reads=[PT2], writes=[xt])

                def compute(ui):
                    kind, k = units[ui]
                    sl = ui % 3
                    ws = wslot(ui)
                    wg, wu, wdn = Wg[ws], Wu[ws], Wd[ws]
                    xt, sg_, at_, aT_ = XT[ui % 2], sgt[ui % 2], at[ui % 2], aT[ui % 2]
                    if ui == 0:
                        stage_a(0)
                    S.group("pe", [mm(P[0][:], xt[:, kc, :], wg[:, kc, :], kc == 0, kc == 15) for kc in range(16)], reads=[xt, wg], writes=[P[0]])
                    S.group("pe", [mm(P[1][:], xt[:, kc, :], wu[:, kc, :], kc == 0, kc == 15) for kc in range(16)], reads=[xt, wu], writes=[P[1]])
                    S.op("act", act(sg_[:], P[0][:], AF.Silu), reads=[P[0]], writes=[sg_])
                    wcol = one_c[:, 0:1] if kind == "s" else rec[sl][:, 1:2]
                    S.op("dve", stt("dve", at_[:], sg_[:], wcol, P[1][:], ALU.mult, ALU.mult), reads=[sg_, P[1], one_c if kind == "s" else rec[sl]], writes=[at_])
                    if ui + 1 < len(units):
                        stage_a(ui + 1)
                    S.group("pe", [tr(PT[:, fc * 128:(fc + 1) * 128], at_[:, fc:512:4]) for fc in range(4)], reads=[at_, ident], writes=[PT])
                    S.op("act", cp("act", aT_[:], PT[:, 0:512].rearrange("p (k t) -> p k t", k=4)), reads=[PT], writes=[aT_])
                    y = yb[ui % 2]
                    for nb in range(4):
                        pb = P[2 + nb]
                        S.group("pe", [mm(pb[:], aT_[:, fc, :], wdn[:, fc, nb * 512:(nb + 1) * 512], fc == 0, fc == 3) for fc in range(4)], reads=[aT_, wdn], writes=[pb])
                        e = "act" if nb % 2 == 0 else "dve"
                        S.op(e, cp(e, y[:, nb * 512:(nb + 1) * 512], pb[:]), reads=[pb], writes=[y])
                    if kind == "s":
                        S.dma("act", dma("act", acc_d[k * 128:(k + 1) * 128, :], y[:]), reads=[y], writes=[B_accd])
                    else:
                        S.dma("pool", (lambda sl=sl, y=y: nc.gpsimd.indirect_dma_start(out=acc_d[:, :], out_offset=bass.IndirectOffsetOnAxis(ap=idx[sl][:, 0:1], axis=0), in_=y[:], in_offset=None, compute_op=ALU.add)),
                              reads=[y, idx[sl], B_accd], writes=[B_accd])

                load(0)
                load(1)
                for ui in range(len(units)):
                    if ui + 2 < len(units):
                        load(ui + 2)
                    compute(ui)

                S.dma("sp", dma("sp", fgt[:], fg_bc[:, :]), writes=[fgt])
                S.dma("sp", dma("sp", gt2_bc[:], gt2_d[:, :]), reads=[B_gt2d], writes=[gt2_bc])
                outbufs = []
                for t in range(4 * n_main_blk):
                    r0 = t * 128
                    S.dma("sp", dma("sp", xa[:], acc_d[r0:r0 + 128, :]), reads=[B_accd], writes=[xa])
                    S.dma("act", dma("act", xb[:], x1_d[r0:r0 + 128, :]), reads=[B_x1d], writes=[xb])
                    S.op("dve", tt("dve", xa[:], xa[:], gt2_bc[:], ALU.mult), reads=[xa, gt2_bc], writes=[xa])
                    S.op("pool", tt("pool", xb[:], xb[:], xa[:], ALU.add), reads=[xa, xb], writes=[xb])
                    S.op("act", act(xa[:], xb[:], AF.Square, accum_out=ss2[:, 0:1]), reads=[xb], writes=[xa, ss2])
                    S.op("act", act(ss2[:, 1:2], ss2[:, 0:1], AF.Sqrt, scale=1.0 / D, bias=eps2[:, 0:1]), reads=[ss2, eps2], writes=[ss2])
                    S.op("dve", lambda: nc.vector.reciprocal(out=ss2[:, 1:2], in_=ss2[:, 1:2]), reads=[ss2], writes=[ss2])
                    S.op("dve", stt("dve", xa[:], xb[:], ss2[:, 1:2], fgt[:], ALU.mult, ALU.mult), reads=[xb, ss2, fgt], writes=[xa])
                    ob = Buf(f"out{t}")
                    outbufs.append(ob)
                    S.dma("sp", dma("sp", out_d[r0:r0 + 128, :], xa[:]), reads=[xa], writes=[ob])
                S.finish([B_out] + outbufs)
                S.emit(block)
    return nc


def _fm(v, k):
    return np.ascontiguousarray(np.asarray(v, np.float32).reshape(k, 128).T)


def make_in_maps(inp):
    f32 = np.float32
    x = np.asarray(inp["x"], f32)
    w_in = np.asarray(inp["w_in"], f32)[0]
    def unit_cols(cols):
        w = w_in[:, cols]
        return np.ascontiguousarray(w.reshape(16, 128, len(cols)).transpose(1, 0, 2))
    w_lru = np.stack([unit_cols(list(range(cb * 128, (cb + 1) * 128)) + list(range(1024 + cb * 128, 1024 + (cb + 1) * 128))) for cb in range(8)])
    w_hg = np.stack([unit_cols(list(range(2048 + h * 128, 2048 + (h + 1) * 128)) + list(range(3072 + h * 128, 3072 + (h + 1) * 128))
                               + list(range(4096 + h * 128, 4096 + (h + 1) * 128)) + list(range(5120 + h * 128, 5120 + (h + 1) * 128))) for h in range(8)])
    conv_w = np.asarray(inp["conv_w"], f32)[0]
    lru_vec = np.zeros((128, 9, 8), f32)
    for j in range(4):
        lru_vec[:, j, :] = _fm(conv_w[j], 8)
    lru_vec[:, 4, :] = _fm(inp["conv_b"][0], 8)
    lru_vec[:, 5, :] = _fm(inp["lru_ba"][0], 8)
    lru_vec[:, 6, :] = _fm(inp["lru_bx"][0], 8)
    lru_vec[:, 7, :] = _fm(inp["lru_lambda"][0], 8)
    lru_wa = np.ascontiguousarray(np.asarray(inp["lru_wa"], f32)[0].transpose(1, 0, 2))
    lru_wx = np.ascontiguousarray(np.asarray(inp["lru_wx"], f32)[0].transpose(1, 0, 2))
    hlb = np.asarray(inp["hgrn_lb"], f32)
    hg_lb = np.stack([_fm(hlb[0], 8), _fm(hlb[1], 8)], axis=1)
    hg_ng = np.ascontiguousarray(np.asarray(inp["hgrn_norm_g"], f32)[0].reshape(128, 1))
    b_ada = np.asarray(inp["b_ada"], f32)[0]
    bada_fm = np.concatenate([_fm(b_ada[0:D], 16), _fm(b_ada[D:2 * D], 16)], axis=1)
    bada_bc = np.stack([np.broadcast_to(b_ada[p * D:(p + 1) * D], (128, D)) for p in (2, 3, 4, 5)]).astype(f32)
    shared = {
        "w_ada": np.ascontiguousarray(np.asarray(inp["w_ada"], f32)[0].reshape(16, 128, 24, 512).transpose(2, 1, 0, 3)), "bada_fm": bada_fm, "bada_bc": np.ascontiguousarray(bada_bc),
        "g1_fm": _fm(inp["norm1_g"][0], 16), "w_lru": w_lru, "w_hg": w_hg, "lru_vec": lru_vec, "lru_wa": lru_wa, "lru_wx": lru_wx,
        "hg_lb": np.ascontiguousarray(hg_lb), "hg_ng": hg_ng, "w_out": np.ascontiguousarray(np.asarray(inp["w_out"], f32)[0].reshape(16, 128, 4, 512).transpose(2, 1, 0, 3)),
        "g2_bc": np.ascontiguousarray(np.broadcast_to(np.asarray(inp["norm2_g"], f32)[0], (128, D))),
        "w_router": np.asarray(inp["w_router"], f32)[0],
        "rb_bc": np.ascontiguousarray(np.broadcast_to(np.asarray(inp["router_bias"], f32)[0], (128, NE))),
        "w_gate": np.asarray(inp["w_gate"], f32)[0], "w_up": np.asarray(inp["w_up"], f32)[0], "w_down": np.asarray(inp["w_down"], f32)[0],
        "ws_gate": np.asarray(inp["ws_gate"], f32)[0], "ws_up": np.asarray(inp["ws_up"], f32)[0], "ws_down": np.asarray(inp["ws_down"], f32)[0],
        "fg_bc": np.ascontiguousarray(np.broadcast_to(np.asarray(inp["final_g"], f32), (128, D))),
    }
    in_maps = []
    for core in range(8):
        b, half = core // 2, core % 2
        xm = x[b, half * NTOK:(half + 1) * NTOK, :]
        xp = x[b, 0:NTOK, :] if half == 1 else np.zeros((NTOK, D), f32)
        fl = np.zeros((128, 2), f32)
        fl[:, 0] = 1.0 if half == 1 else 0.0
        fl[:, 1] = 0.0 if half == 1 else 1.0
        m = dict(shared)
        m.update({"xT_pre": np.ascontiguousarray(xp.T), "xT_main": np.ascontiguousarray(xm.T), "x_tok": np.ascontiguousarray(xm),
                  "flags": fl, "c_fm": _fm(np.asarray(inp["c"], f32)[b], 16)})
        in_maps.append(m)
    return in_maps


def kernel(**inputs):
    in_maps = make_in_maps(inputs)
    nc = build_nc()
    res = run_bass_kernel_spmd(nc, in_maps, core_ids=list(range(8)))
    out = np.zeros((4, 2 * NTOK, D), np.float32)
    for core in range(8):
        b, half = core // 2, core % 2
        out[b, half * NTOK:(half + 1) * NTOK, :] = res.results[core]["out"]
    return out
```
